# Optimizing a Trainium2 kernel written in Bass

```python
import jax, jax.numpy as jnp
from jax import lax
import numpy as np

D_MODEL = 4096
BATCH = 1
SEQ = 8192
DEPTH = 4

CTX_LEN = 256
GRID_W = 64
N_MIXERS = 3
Q_BLOCK = 128
ROPE_THETA = 10000.0
EPS = 1e-6
N_MOD = 6

NA_HEADS = 32
NA_HEAD_DIM = D_MODEL // NA_HEADS
NA_WIN_H = 8
NA_WIN_W = 16

MLA_HEADS = 32
MLA_Q_RANK = 1024
MLA_KV_RANK = 512
MLA_NOPE = 128
MLA_ROPE = 64
MLA_V = 128

GQA_HEADS = 32
GQA_KV_HEADS = 8
GQA_HEAD_DIM = 128

PEER_HEADS = 8
PEER_N_KEYS = 96
PEER_N_EXPERTS = PEER_N_KEYS * PEER_N_KEYS
PEER_TOPK = 16
PEER_KEY_DIM = 256
PEER_HALF = PEER_KEY_DIM // 2
PEER_CHUNK = 64

DEEPNORM_ALPHA = (2 * DEPTH) ** 0.25
DEEPNORM_BETA = (8 * DEPTH) ** -0.25

kernel_name = "hybrid_natten_mla_gqa_peer_prefix_dit"


def layer_norm(x, g, b):
    xf = x.astype(jnp.float32)
    mu = jnp.mean(xf, -1, keepdims=True)
    var = jnp.mean(jnp.square(xf - mu), -1, keepdims=True)
    y = (xf - mu) * lax.rsqrt(var + 1e-5) * g.astype(jnp.float32) + b.astype(jnp.float32)
    return y.astype(x.dtype)


def rms_norm(x, g):
    xf = x.astype(jnp.float32)
    y = xf * lax.rsqrt(jnp.mean(jnp.square(xf), -1, keepdims=True) + EPS)
    return (y * g.astype(jnp.float32)).astype(x.dtype)


def rope_1d(x, pos):
    half = x.shape[-1] // 2
    inv = ROPE_THETA ** (-jnp.arange(half, dtype=jnp.float32) / half)
    ang = pos.astype(jnp.float32)[:, None] * inv[None, :]
    cos = jnp.cos(ang)[None, :, None, :]
    sin = jnp.sin(ang)[None, :, None, :]
    xf = x.astype(jnp.float32)
    x1, x2 = xf[..., :half], xf[..., half:]
    return jnp.concatenate([x1 * cos - x2 * sin, x1 * sin + x2 * cos], -1).astype(x.dtype)


def rope_2d(x, rows, cols):
    h = x.shape[-1] // 2
    return jnp.concatenate([rope_1d(x[..., :h], rows), rope_1d(x[..., h:], cols)], -1)


def to_blocks(a):
    b, s = a.shape[:2]
    return jnp.moveaxis(a.reshape(b, s // Q_BLOCK, Q_BLOCK, *a.shape[2:]), 1, 0)


def from_blocks(a):
    a = jnp.moveaxis(a, 0, 1)
    return a.reshape(a.shape[0], a.shape[1] * a.shape[2], *a.shape[3:])


def dense_attention(q, k, v, scale):
    b, _, hq, d = q.shape
    hkv = k.shape[2]
    g = hq // hkv
    dv = v.shape[-1]

    def block(qb):
        qb = qb.reshape(b, Q_BLOCK, hkv, g, d)
        s = jnp.einsum('bqkgd,bskd->bkgqs', qb, k).astype(jnp.float32) * scale
        p = jax.nn.softmax(s, axis=-1).astype(v.dtype)
        o = jnp.einsum('bkgqs,bskd->bqkgd', p, v)
        return o.reshape(b, Q_BLOCK, hq, dv)

    return from_blocks(lax.map(block, to_blocks(q)))


def neighbourhood_tables(s):
    rows = s // GRID_W
    kh = min(NA_WIN_H, rows)
    kw = NA_WIN_W
    t = jnp.arange(s, dtype=jnp.int32)
    r, col = t // GRID_W, t % GRID_W
    sr = jnp.clip(r - kh // 2, 0, rows - kh)
    sc = jnp.clip(col - kw // 2, 0, GRID_W - kw)
    kr = sr[:, None] + jnp.arange(kh, dtype=jnp.int32)
    kc = sc[:, None] + jnp.arange(kw, dtype=jnp.int32)
    idx = (kr[:, :, None] * GRID_W + kc[:, None, :]).reshape(s, kh * kw)
    dr = kr - r[:, None] + (NA_WIN_H - 1)
    dc = kc - col[:, None] + (NA_WIN_W - 1)
    bidx = (dr[:, :, None] * (2 * NA_WIN_W - 1) + dc[:, None, :]).reshape(s, kh * kw)
    return idx, bidx


def natten_mixer(hc, hl, w_qkv, rpb, w_o, nbr_idx, bias_idx, need_ctx):
    b = hl.shape[0]

    def proj(h):
        qkv = (h @ w_qkv).reshape(b, h.shape[1], 3, NA_HEADS, NA_HEAD_DIM)
        return qkv[:, :, 0], qkv[:, :, 1], qkv[:, :, 2]

    qc, kc, vc = proj(hc)
    ql, kl, vl = proj(hl)
    scale = NA_HEAD_DIM ** -0.5
    rpb_flat = rpb.reshape(NA_HEADS, -1).astype(jnp.float32)
    n_loc = nbr_idx.shape[1]

    def block(args):
        qb, ib, bb = args
        kg = kl[:, ib]
        vg = vl[:, ib]
        s_loc = jnp.einsum('bqhd,bqkhd->bhqk', qb, kg).astype(jnp.float32) * scale + rpb_flat[:, bb][None]
        s_ctx = jnp.einsum('bqhd,bchd->bhqc', qb, kc).astype(jnp.float32) * scale
        p = jax.nn.softmax(jnp.concatenate([s_loc, s_ctx], -1), axis=-1).astype(vl.dtype)
        return (jnp.einsum('bhqk,bqkhd->bqhd', p[..., :n_loc], vg)
                + jnp.einsum('bhqc,bchd->bqhd', p[..., n_loc:], vc))

    nb = ql.shape[1] // Q_BLOCK
    ol = from_blocks(lax.map(block, (to_blocks(ql),
                                     nbr_idx.reshape(nb, Q_BLOCK, n_loc),
                                     bias_idx.reshape(nb, Q_BLOCK, n_loc))))
    yl = ol.reshape(b, -1, NA_HEADS * NA_HEAD_DIM) @ w_o
    yc = None
    if need_ctx:
        yc = dense_attention(qc, kc, vc, scale).reshape(b, -1, NA_HEADS * NA_HEAD_DIM) @ w_o
    return yc, yl


def mla_mixer(hc, hl, w_dq, q_norm, w_uq, w_dkv, kv_norm, w_ukv, w_o, rows, cols, need_ctx):
    b = hl.shape[0]

    def proj(h, rotate):
        t = h.shape[1]
        q = (rms_norm(h @ w_dq, q_norm) @ w_uq).reshape(b, t, MLA_HEADS, MLA_NOPE + MLA_ROPE)
        q_nope, q_pe = q[..., :MLA_NOPE], q[..., MLA_NOPE:]
        ckv = h @ w_dkv
        c_kv, k_pe = ckv[..., :MLA_KV_RANK], ckv[..., MLA_KV_RANK:][:, :, None, :]
        kv = (rms_norm(c_kv, kv_norm) @ w_ukv).reshape(b, t, MLA_HEADS, MLA_NOPE + MLA_V)
        k_nope, v = kv[..., :MLA_NOPE], kv[..., MLA_NOPE:]
        if rotate:
            q_pe = rope_2d(q_pe, rows, cols)
            k_pe = rope_2d(k_pe, rows, cols)
        q = jnp.concatenate([q_nope, q_pe], -1)
        k = jnp.concatenate([k_nope, jnp.broadcast_to(k_pe, (b, t, MLA_HEADS, MLA_ROPE))], -1)
        return q, k, v

    qc, kc, vc = proj(hc, False)
    ql, kl, vl = proj(hl, True)
    scale = (MLA_NOPE + MLA_ROPE) ** -0.5
    ol = dense_attention(ql, jnp.concatenate([kl, kc], 1), jnp.concatenate([vl, vc], 1), scale)
    yl = ol.reshape(b, -1, MLA_HEADS * MLA_V) @ w_o
    yc = None
    if need_ctx:
        yc = dense_attention(qc, kc, vc, scale).reshape(b, -1, MLA_HEADS * MLA_V) @ w_o
    return yc, yl


def gqa_mixer(hc, hl, w_q, w_k, w_v, q_norm, k_norm, w_o, rows, cols, need_ctx):
    b = hl.shape[0]

    def proj(h, rotate):
        t = h.shape[1]
        q = rms_norm((h @ w_q).reshape(b, t, GQA_HEADS, GQA_HEAD_DIM), q_norm)
        k = rms_norm((h @ w_k).reshape(b, t, GQA_KV_HEADS, GQA_HEAD_DIM), k_norm)
        v = (h @ w_v).reshape(b, t, GQA_KV_HEADS, GQA_HEAD_DIM)
        if rotate:
            q = rope_2d(q, rows, cols)
            k = rope_2d(k, rows, cols)
        return q, k, v

    qc, kc, vc = proj(hc, False)
    ql, kl, vl = proj(hl, True)
    scale = GQA_HEAD_DIM ** -0.5
    ol = dense_attention(ql, jnp.concatenate([kl, kc], 1), jnp.concatenate([vl, vc], 1), scale)
    yl = ol.reshape(b, -1, GQA_HEADS * GQA_HEAD_DIM) @ w_o
    yc = None
    if need_ctx:
        yc = dense_attention(qc, kc, vc, scale).reshape(b, -1, GQA_HEADS * GQA_HEAD_DIM) @ w_o
    return yc, yl


def peer_ffn(h, w_query, sub_keys, u, v):
    b, t, d = h.shape
    n = b * t
    hf = h.reshape(n, d)
    q = (hf @ w_query).reshape(n, PEER_HEADS, 2, PEER_HALF)
    s = jnp.einsum('nphd,phkd->nphk', q, sub_keys).astype(jnp.float32)
    s1, i1 = lax.top_k(s[:, :, 0], PEER_TOPK)
    s2, i2 = lax.top_k(s[:, :, 1], PEER_TOPK)
    cand = (s1[..., :, None] + s2[..., None, :]).reshape(n, PEER_HEADS, PEER_TOPK * PEER_TOPK)
    best, bi = lax.top_k(cand, PEER_TOPK)
    expert = (jnp.take_along_axis(i1, bi // PEER_TOPK, -1) * PEER_N_KEYS
              + jnp.take_along_axis(i2, bi % PEER_TOPK, -1))
    gate = jax.nn.softmax(best, axis=-1)

    def chunk(args):
        xc, ec, gc = args
        a = jnp.einsum('cd,cpkd->cpk', xc, u[ec]).astype(jnp.float32)
        w = (jax.nn.gelu(a, approximate=False) * gc).astype(xc.dtype)
        return jnp.einsum('cpk,cpkd->cd', w, v[ec])

    nc = n // PEER_CHUNK
    y = lax.map(chunk, (hf.reshape(nc, PEER_CHUNK, d),
                        expert.reshape(nc, PEER_CHUNK, PEER_HEADS, PEER_TOPK),
                        gate.reshape(nc, PEER_CHUNK, PEER_HEADS, PEER_TOPK)))
    return y.reshape(b, t, d)


def setup_inputs(seed: int = 0) -> dict:
    key = jax.random.key(seed)
    ks = iter(jax.random.split(key, 40))

    def nrm(shape, scale):
        return jax.random.normal(next(ks), shape, jnp.float32) * scale

    n_a = len(range(0, DEPTH, N_MIXERS))
    n_b = len(range(1, DEPTH, N_MIXERS))
    n_c = len(range(2, DEPTH, N_MIXERS))
    D = D_MODEL
    beta = DEEPNORM_BETA
    return {
        "x": nrm((BATCH, SEQ, D), 1.0),
        "c": nrm((BATCH, D), 1.0),
        "ctx": nrm((BATCH, CTX_LEN, D), 1.0),
        "c_ctx": nrm((D,), 1.0),
        "ada_w": nrm((DEPTH, D, N_MOD * D), 0.5 * D ** -0.5),
        "ada_b": nrm((DEPTH, N_MOD * D), 0.01),
        "ln_g": 1.0 + nrm((DEPTH, 2, D), 0.02),
        "ln_b": nrm((DEPTH, 2, D), 0.02),
        "na_w_qkv": nrm((n_a, D, 3 * NA_HEADS * NA_HEAD_DIM), D ** -0.5),
        "na_rpb": nrm((n_a, NA_HEADS, 2 * NA_WIN_H - 1, 2 * NA_WIN_W - 1), 0.1),
        "na_w_o": nrm((n_a, NA_HEADS * NA_HEAD_DIM, D), beta * (NA_HEADS * NA_HEAD_DIM) ** -0.5),
        "mla_w_dq": nrm((n_b, D, MLA_Q_RANK), D ** -0.5),
        "mla_q_norm": 1.0 + nrm((n_b, MLA_Q_RANK), 0.02),
        "mla_w_uq": nrm((n_b, MLA_Q_RANK, MLA_HEADS * (MLA_NOPE + MLA_ROPE)), MLA_Q_RANK ** -0.5),
        "mla_w_dkv": nrm((n_b, D, MLA_KV_RANK + MLA_ROPE), D ** -0.5),
        "mla_kv_norm": 1.0 + nrm((n_b, MLA_KV_RANK), 0.02),
        "mla_w_ukv": nrm((n_b, MLA_KV_RANK, MLA_HEADS * (MLA_NOPE + MLA_V)), MLA_KV_RANK ** -0.5),
        "mla_w_o": nrm((n_b, MLA_HEADS * MLA_V, D), beta * (MLA_HEADS * MLA_V) ** -0.5),
        "gqa_w_q": nrm((n_c, D, GQA_HEADS * GQA_HEAD_DIM), D ** -0.5),
        "gqa_w_k": nrm((n_c, D, GQA_KV_HEADS * GQA_HEAD_DIM), D ** -0.5),
        "gqa_w_v": nrm((n_c, D, GQA_KV_HEADS * GQA_HEAD_DIM), D ** -0.5),
        "gqa_q_norm": 1.0 + nrm((n_c, GQA_HEAD_DIM), 0.02),
        "gqa_k_norm": 1.0 + nrm((n_c, GQA_HEAD_DIM), 0.02),
        "gqa_w_o": nrm((n_c, GQA_HEADS * GQA_HEAD_DIM, D), beta * (GQA_HEADS * GQA_HEAD_DIM) ** -0.5),
        "peer_w_query": nrm((DEPTH, D, PEER_HEADS * PEER_KEY_DIM), D ** -0.5),
        "peer_sub_keys": nrm((DEPTH, PEER_HEADS, 2, PEER_N_KEYS, PEER_HALF), PEER_HALF ** -0.5),
        "peer_u": nrm((DEPTH, PEER_N_EXPERTS, D), D ** -0.5),
        "peer_v": nrm((DEPTH, PEER_N_EXPERTS, D), beta),
    }


def reference(x, c, ctx, c_ctx, ada_w, ada_b, ln_g, ln_b,
              na_w_qkv, na_rpb, na_w_o,
              mla_w_dq, mla_q_norm, mla_w_uq, mla_w_dkv, mla_kv_norm, mla_w_ukv, mla_w_o,
              gqa_w_q, gqa_w_k, gqa_w_v, gqa_q_norm, gqa_k_norm, gqa_w_o,
              peer_w_query, peer_sub_keys, peer_u, peer_v):
    s = x.shape[1]
    t = jnp.arange(s, dtype=jnp.int32)
    rows, cols = t // GRID_W, t % GRID_W
    nbr_idx, bias_idx = neighbourhood_tables(s)
    n_ctx = ctx.shape[1]

    cond_l = jax.nn.silu(c)
    cond_c = jax.nn.silu(c_ctx)[None, :]
    xl, xc = x, ctx
    for i in range(DEPTH):
        kind, j = i % N_MIXERS, i // N_MIXERS
        need_ctx = i < DEPTH - 1
        sh_l, sc_l, g_l, fsh_l, fsc_l, fg_l = jnp.split((cond_l @ ada_w[i] + ada_b[i])[:, None, :], N_MOD, -1)
        sh_c, sc_c, g_c, fsh_c, fsc_c, fg_c = jnp.split((cond_c @ ada_w[i] + ada_b[i])[:, None, :], N_MOD, -1)

        hl = xl * (1 + sc_l) + sh_l
        hc = xc * (1 + sc_c) + sh_c
        if kind == 0:
            yc, yl = natten_mixer(hc, hl, na_w_qkv[j], na_rpb[j], na_w_o[j], nbr_idx, bias_idx, need_ctx)
        elif kind == 1:
            yc, yl = mla_mixer(hc, hl, mla_w_dq[j], mla_q_norm[j], mla_w_uq[j], mla_w_dkv[j],
                               mla_kv_norm[j], mla_w_ukv[j], mla_w_o[j], rows, cols, need_ctx)
        else:
            yc, yl = gqa_mixer(hc, hl, gqa_w_q[j], gqa_w_k[j], gqa_w_v[j], gqa_q_norm[j],
                               gqa_k_norm[j], gqa_w_o[j], rows, cols, need_ctx)
        xl = layer_norm(DEEPNORM_ALPHA * xl + g_l * yl, ln_g[i, 0], ln_b[i, 0])

        if need_ctx:
            xc = layer_norm(DEEPNORM_ALPHA * xc + g_c * yc, ln_g[i, 0], ln_b[i, 0])
            h = jnp.concatenate([xc * (1 + fsc_c) + fsh_c, xl * (1 + fsc_l) + fsh_l], 1)
            f = peer_ffn(h, peer_w_query[i], peer_sub_keys[i], peer_u[i], peer_v[i])
            fc, fl = f[:, :n_ctx], f[:, n_ctx:]
            xc = layer_norm(DEEPNORM_ALPHA * xc + fg_c * fc, ln_g[i, 1], ln_b[i, 1])
        else:
            fl = peer_ffn(xl * (1 + fsc_l) + fsh_l, peer_w_query[i], peer_sub_keys[i], peer_u[i], peer_v[i])
        xl = layer_norm(DEEPNORM_ALPHA * xl + fg_l * fl, ln_g[i, 1], ln_b[i, 1])
    return xl
```

```python
import numpy as np
import ml_dtypes
import concourse.bass as bass
import concourse.mybir as mybir
from concourse.bass_utils import run_bass_kernel_spmd

F32 = mybir.dt.float32
BF16 = mybir.dt.bfloat16
AF = mybir.ActivationFunctionType
ALU = mybir.AluOpType
AX = mybir.AxisListType

SEM_LIMIT = 65000


class Tk:
    __slots__ = ("name", "w", "r", "shared")

    def __init__(self, name="", shared=False):
        self.name = name
        self.w = {}
        self.r = {}
        self.shared = shared

    def wrote(self, ref):
        s, v = ref
        if self.shared:
            if self.w.get(s, 0) < v:
                self.w[s] = v
        else:
            self.w = {s: v}
            self.r = {}

    def readby(self, ref):
        s, v = ref
        if self.r.get(s, 0) < v:
            self.r[s] = v


class Sched:
    ENG = ("sp", "act", "dve", "pe", "pool")

    def __init__(self, nc):
        self.nc = nc
        self.lists = {e: [] for e in self.ENG}
        self.sems = {}
        self.nsem = 0
        self.cur = {}
        self.dma = {}
        self.dma_rr = {}
        self.wm = {e: {} for e in self.ENG}
        self.nops = 0
        for e in ("act", "dve", "pe", "pool"):
            self.cur[e] = [self._new_sem(), 0]
        for q in ("sp", "pool", "act"):
            self.dma[q] = [[self._new_sem(), 0] for _ in range(6)]
            self.dma_rr[q] = 0

    def _new_sem(self):
        sid = self.nsem
        self.nsem += 1
        self.sems[sid] = self.nc.alloc_semaphore(f"s{sid}")
        return sid

    def _deps(self, eng, reads, writes, selfsem=None):
        deps = {}
        def add(dd):
            for s, v in dd.items():
                if deps.get(s, 0) < v:
                    deps[s] = v
        for t in reads:
            add(t.w)
        for t in writes:
            add(t.w)
            add(t.r)
        waits = []
        wm = self.wm[eng]
        for s, v in deps.items():
            if eng == "pe" and selfsem == s:
                continue
            if wm.get(s, 0) >= v:
                continue
            wm[s] = v
            waits.append((s, v))
        return waits

    def op(self, eng, fn, reads=(), writes=()):
        cs = self.cur[eng]
        if cs[1] >= SEM_LIMIT:
            cs = self.cur[eng] = [self._new_sem(), 0]
        waits = self._deps(eng, reads, writes, selfsem=cs[0])
        cs[1] += 1
        sid, val = cs[0], cs[1]
        sems = self.sems
        def run(e, waits=waits, fn=fn, sid=sid):
            for s, v in waits:
                e.wait_ge(sems[s], v)
            fn(e).then_inc(sems[sid], 1)
        self.lists[eng].append(run)
        ref = (sid, val)
        for t in writes:
            t.wrote(ref)
        for t in reads:
            t.readby(ref)
        self.nops += 1

    def dma_op(self, q, fn, reads=(), writes=()):
        lst = self.dma[q]
        i = self.dma_rr[q]
        self.dma_rr[q] = (i + 1) % len(lst)
        cs = lst[i]
        if cs[1] + 16 > SEM_LIMIT:
            cs = lst[i] = [self._new_sem(), 0]
        waits = self._deps(q, reads, writes)
        cs[1] += 16
        sid, val = cs[0], cs[1]
        sems = self.sems
        def run(e, waits=waits, fn=fn, sid=sid):
            for s, v in waits:
                e.wait_ge(sems[s], v)
            fn(e).then_inc(sems[sid], 16)
        self.lists[q].append(run)
        ref = (sid, val)
        for t in writes:
            t.wrote(ref)
        for t in reads:
            t.readby(ref)
        self.nops += 1
        return ref

    def final_wait(self, tks):
        deps = {}
        for t in tks:
            for s, v in t.w.items():
                deps[s] = max(deps.get(s, 0), v)
        sems = self.sems
        def run(e, deps=deps):
            for s, v in deps.items():
                e.wait_ge(sems[s], v)
        self.lists["sp"].append(run)

    def emit(self):
        nc = self.nc
        with nc.Block() as block:
            L = self.lists
            @block.sync
            def _(e):
                for f in L["sp"]:
                    f(e)
            @block.scalar
            def _(e):
                for f in L["act"]:
                    f(e)
            @block.vector
            def _(e):
                for f in L["dve"]:
                    f(e)
            @block.tensor
            def _(e):
                for f in L["pe"]:
                    f(e)
            @block.gpsimd
            def _(e):
                for f in L["pool"]:
                    f(e)


class Buf:
    def __init__(self, nc, name, shape, dt, psum=False):
        if psum:
            self.t = nc.alloc_psum_tensor(name, shape, dt)
        else:
            self.t = nc.alloc_sbuf_tensor(name, shape, dt)
        self.k = Tk(name)
        self.shape = shape

    def __getitem__(self, idx):
        return self.t[idx]


class Ring:
    def __init__(self, nc, name, shape, dt, n, psum=False):
        self.bufs = [Buf(nc, f"{name}{i}", shape, dt, psum) for i in range(n)]
        self.i = 0

    def next(self):
        b = self.bufs[self.i]
        self.i = (self.i + 1) % len(self.bufs)
        return b


D = 4096
KC = 32
NCTX = 256
ALPHA = 8 ** 0.25


class View:
    def __init__(self, ap, k):
        self.t = ap
        self.k = k

    def __getitem__(self, idx):
        return self.t[idx]


class VRing:
    def __init__(self, views):
        self.bufs = views
        self.i = 0

    def next(self):
        b = self.bufs[self.i]
        self.i = (self.i + 1) % len(self.bufs)
        return b


def carve(buf, dt, specs):
    flat = buf.t[:].rearrange("p a b -> p (a b)")
    if dt == F32:
        flat = flat.bitcast(F32)
    off = 0
    out = []
    for shape in specs:
        n = int(np.prod(shape))
        v = flat[:, off:off + n]
        if len(shape) == 2:
            v = v.rearrange("p (a b) -> p a b", a=shape[0])
        elif len(shape) == 3:
            v = v.rearrange("p (a b c) -> p a b c", a=shape[0], b=shape[1])
        elif len(shape) == 4:
            v = v.rearrange("p (a b c d) -> p a b c d", a=shape[0], b=shape[1], c=shape[2])
        out.append(View(v, buf.k))
        off += n
    assert off * (4 if dt == F32 else 2) <= 32768, off
    return out


class Ctx:
    def __init__(self, nc, T):
        self.nc = nc
        self.T = T
        self.S = Sched(nc)
        self.P = {
            "w": Ring(nc, "w", [128, KC, 512], BF16, 2),
            "x": Ring(nc, "x", [128, KC, 512], BF16, 2),
            "o": Ring(nc, "o", [128, 512], F32, 3),
            "ob": Ring(nc, "ob", [128, 512], BF16, 3),
            "xr": Ring(nc, "xr", [128, 512], F32, 3),
        }
        self.psA = nc.alloc_psum_tensor("psA", [128, 4, 512], F32)
        self.psB = nc.alloc_psum_tensor("psB", [128, 4, 512], F32)
        self.P["ps"] = VRing([View(self.psA[:, j, :], Tk(f"psA{j}")) for j in range(4)])
        self.P["pacc"] = VRing([View(self.psB[:, j, :], Tk(f"psB{j}")) for j in range(4)])
        self.mod = Buf(nc, "mod", [128, 4, 192, 2], F32)
        self.dk = {}

    def dram(self, name, shape, dt):
        t = self.nc.dram_tensor(name, shape, dt).ap()
        self.dk[name] = Tk(name, True)
        return t

    def tiles(self, TT=512):
        out = []
        t = 0
        while t < NCTX:
            tt = min(TT, NCTX - t)
            out.append((t, tt, 1))
            t += tt
        while t < self.T:
            tt = min(TT, self.T - t)
            out.append((t, tt, 0))
            t += tt
        return out


def ada_stage(C, ada_w, ada_bT, ccT, layers=4):
    S, nc, P = C.S, C.nc, C.P
    cc = Buf(nc, "cc", [128, KC, 2], F32)
    ccb = Buf(nc, "ccb", [128, KC, 2], BF16)
    bT = Buf(nc, "bT", [128, 4, 192], F32)
    src = Tk("in", True)
    S.dma_op("sp", lambda e: e.dma_start(out=cc[:], in_=ccT), reads=[src], writes=[cc.k])
    S.dma_op("sp", lambda e: e.dma_start(out=bT[:], in_=ada_bT), reads=[src], writes=[bT.k])
    S.op("act", lambda e: e.activation(out=ccb[:], in_=cc[:], func=AF.Silu), reads=[cc.k], writes=[ccb.k])
    for i in range(layers):
        wv = ada_w[i].rearrange("(kc p) n -> p kc n", p=128)
        for n0 in range(0, 6 * D, 512):
            wb = P["w"].next()
            S.dma_op("pool", lambda e, wb=wb, wv=wv, n0=n0: e.dma_start(out=wb[:, :, :], in_=wv[:, :, n0:n0 + 512]), reads=[src], writes=[wb.k])
            for j in range(4):
                ch = n0 // 128 + j
                ps = P["ps"].next()
                for kc in range(KC):
                    S.op("pe", lambda e, ps=ps, wb=wb, kc=kc, j=j: e.matmul(ps[:, 0:2], lhsT=wb[:, kc, j * 128:(j + 1) * 128], rhs=ccb[:, kc, :], start=(kc == 0), stop=(kc == KC - 1)),
                         reads=[wb.k, ccb.k], writes=[ps.k])
                v = ch // 32
                if v in (1, 4):
                    S.op("dve", lambda e, ps=ps, i=i, ch=ch: e.tensor_scalar(out=C.mod[:, i, ch, :], in0=ps[:, 0:2], scalar1=bT[:, i, ch:ch + 1], scalar2=1.0, op0=ALU.add, op1=ALU.add),
                         reads=[ps.k, bT.k], writes=[C.mod.k])
                else:
                    S.op("dve", lambda e, ps=ps, i=i, ch=ch: e.tensor_scalar(out=C.mod[:, i, ch, :], in0=ps[:, 0:2], scalar1=bT[:, i, ch:ch + 1], scalar2=None, op0=ALU.add),
                         reads=[ps.k, bT.k], writes=[C.mod.k])


def modulate(C, xT, xname, hT, hname, i, vsh, vsc):
    S, P = C.S, C.P
    xv = xT.rearrange("(kc p) t -> p kc t", p=128)
    hv = hT.rearrange("(kc p) t -> p kc t", p=128)
    for (t0, tt, col) in C.tiles(128):
        for half in range(2):
            xb = P["xf"].next()
            hb = P["x"].next()
            k0 = half * 16
            S.dma_op("sp", lambda e, xb=xb, t0=t0, tt=tt, k0=k0: e.dma_start(out=xb[:, :, :tt], in_=xv[:, k0:k0 + 16, t0:t0 + tt]), reads=[C.dk[xname]], writes=[xb.k])
            for kk in range(16):
                kc = k0 + kk
                S.op("dve", lambda e, xb=xb, hb=hb, kk=kk, kc=kc, tt=tt, col=col: e.tensor_scalar(
                    out=hb[:, kk, :tt], in0=xb[:, kk, :tt], scalar1=C.mod[:, i, vsc * 32 + kc, col:col + 1], scalar2=C.mod[:, i, vsh * 32 + kc, col:col + 1], op0=ALU.mult, op1=ALU.add),
                    reads=[xb.k, C.mod.k], writes=[hb.k])
            S.dma_op("act", lambda e, hb=hb, t0=t0, tt=tt, k0=k0: e.dma_start(out=hv[:, k0:k0 + 16, t0:t0 + tt], in_=hb[:, 0:16, :tt]), reads=[hb.k], writes=[C.dk[hname]])


def linear_fm(C, xT, xname, W, yT, yname, K, N, ybf16=True, epi=None, tiles=None):
    S, P = C.S, C.P
    kcn = K // 128
    xv = xT.rearrange("(kc p) t -> p kc t", p=128)
    wv = W.rearrange("(kc p) n -> p kc n", p=128)
    src = Tk("in", True)
    tl = tiles if tiles is not None else C.tiles(512)
    for n0 in range(0, N, 512):
        ng = min(512, N - n0)
        wb = P["w"].next()
        S.dma_op("pool", lambda e, wb=wb, n0=n0, ng=ng: e.dma_start(out=wb[:, :kcn, :ng], in_=wv[:, :, n0:n0 + ng]), reads=[src], writes=[wb.k])
        for (t0, tt, col) in tl:
            xb = P["x"].next()
            S.dma_op("sp", lambda e, xb=xb, t0=t0, tt=tt: e.dma_start(out=xb[:, :kcn, :tt], in_=xv[:, :, t0:t0 + tt]), reads=[C.dk[xname]], writes=[xb.k])
            for j in range((ng + 127) // 128):
                m = min(128, ng - j * 128)
                ps = P["ps"].next()
                for kc in range(kcn):
                    S.op("pe", lambda e, ps=ps, wb=wb, xb=xb, kc=kc, j=j, tt=tt, m=m: e.matmul(ps[:m, :tt], lhsT=wb[:, kc, j * 128:j * 128 + m], rhs=xb[:, kc, :tt], start=(kc == 0), stop=(kc == kcn - 1)),
                         reads=[wb.k, xb.k], writes=[ps.k])
                r0 = n0 + j * 128
                if epi is not None:
                    epi(ps, r0, t0, tt, col)
                    continue
                ob = P["ob" if ybf16 else "o"].next()
                S.op("act", lambda e, ob=ob, ps=ps, tt=tt, m=m: e.activation(out=ob[:m, :tt], in_=ps[:m, :tt], func=AF.Copy), reads=[ps.k], writes=[ob.k])
                S.dma_op("sp", lambda e, ob=ob, r0=r0, t0=t0, tt=tt, m=m: e.dma_start(out=yT[r0:r0 + m, t0:t0 + tt], in_=ob[:m, :tt]), reads=[ob.k], writes=[C.dk[yname]])


def linear_tm(C, xT, xname, W, y, yname, K, N):
    S, P = C.S, C.P
    kcn = K // 128
    xv = xT.rearrange("(kc p) t -> p kc t", p=128)
    wv = W.rearrange("(kc p) n -> p kc n", p=128)
    src = Tk("in", True)
    for n0 in range(0, N, 512):
        ng = min(512, N - n0)
        wb = P["w"].next()
        S.dma_op("pool", lambda e, wb=wb, n0=n0, ng=ng: e.dma_start(out=wb[:, :kcn, :ng], in_=wv[:, :, n0:n0 + ng]), reads=[src], writes=[wb.k])
        for (t0, tt, col) in C.tiles(512):
            xb = P["x"].next()
            S.dma_op("sp", lambda e, xb=xb, t0=t0, tt=tt: e.dma_start(out=xb[:, :kcn, :tt], in_=xv[:, :, t0:t0 + tt]), reads=[C.dk[xname]], writes=[xb.k])
            for m in range(tt // 128):
                ps = P["ps"].next()
                for kc in range(kcn):
                    S.op("pe", lambda e, ps=ps, wb=wb, xb=xb, kc=kc, m=m, ng=ng: e.matmul(ps[:, :ng], lhsT=xb[:, kc, m * 128:(m + 1) * 128], rhs=wb[:, kc, :ng], start=(kc == 0), stop=(kc == kcn - 1)),
                         reads=[wb.k, xb.k], writes=[ps.k])
                ob = P["ob"].next()
                S.op("act", lambda e, ob=ob, ps=ps, ng=ng: e.activation(out=ob[:, :ng], in_=ps[:, :ng], func=AF.Copy), reads=[ps.k], writes=[ob.k])
                r0 = t0 + m * 128
                S.dma_op("sp", lambda e, ob=ob, r0=r0, n0=n0, ng=ng: e.dma_start(out=y[r0:r0 + 128, n0:n0 + ng], in_=ob[:, :ng]), reads=[ob.k], writes=[C.dk[yname]])


def resid_epi(C, xT, xname, zT, zname, i, vg):
    S, P = C.S, C.P
    def epi(ps, r0, t0, tt, col):
        ch = r0 // 128
        xr = P["xr"].next()
        S.dma_op("sp", lambda e: e.dma_start(out=xr[:, :tt], in_=xT[r0:r0 + 128, t0:t0 + tt]), reads=[C.dk[xname]], writes=[xr.k])
        S.op("act", lambda e: e.activation(out=xr[:, :tt], in_=xr[:, :tt], func=AF.Copy, scale=float(ALPHA)), reads=[xr.k], writes=[xr.k])
        ob = P["o"].next()
        S.op("dve", lambda e: e.scalar_tensor_tensor(out=ob[:, :tt], in0=ps[:, :tt], scalar=C.mod[:, i, vg * 32 + ch, col:col + 1], in1=xr[:, :tt], op0=ALU.mult, op1=ALU.add),
             reads=[ps.k, xr.k, C.mod.k], writes=[ob.k])
        S.dma_op("sp", lambda e: e.dma_start(out=zT[r0:r0 + 128, t0:t0 + tt], in_=ob[:, :tt]), reads=[ob.k], writes=[C.dk[zname]])
    return epi


def layernorm(C, zT, zname, xoT, xoname, gT, bT, hT=None, hname=None, i=0, vsh=3, vsc=4):
    S, P, nc = C.S, C.P, C.nc
    zv = zT.rearrange("(kc p) t -> p kc t", p=128)
    xov = xoT.rearrange("(kc p) t -> p kc t", p=128)
    hv = hT.rearrange("(kc p) t -> p kc t", p=128) if hT is not None else None
    TT = 256
    for (t0, tt, col) in C.tiles(TT):
        zb = P["lnz"].next()
        sq = P["lnq"].next()
        S.dma_op("sp", lambda e, zb=zb, t0=t0, tt=tt: e.dma_start(out=zb[:, :, :tt], in_=zv[:, :, t0:t0 + tt]), reads=[C.dk[zname]], writes=[zb.k])
        S.op("act", lambda e, zb=zb, sq=sq, tt=tt: e.activation(out=sq[:, :, :tt], in_=zb[:, :, :tt], func=AF.Square), reads=[zb.k], writes=[sq.k])
        pm = P["ps"].next()
        pq = P["ps"].next()
        for kc in range(KC):
            S.op("pe", lambda e, pm=pm, zb=zb, kc=kc, tt=tt: e.matmul(pm[:, :tt], lhsT=C.onesD[:, :], rhs=zb[:, kc, :tt], start=(kc == 0), stop=(kc == KC - 1)), reads=[zb.k, C.onesD.k], writes=[pm.k])
        for kc in range(KC):
            S.op("pe", lambda e, pq=pq, sq=sq, kc=kc, tt=tt: e.matmul(pq[:, :tt], lhsT=C.onesD[:, :], rhs=sq[:, kc, :tt], start=(kc == 0), stop=(kc == KC - 1)), reads=[sq.k, C.onesD.k], writes=[pq.k])
        st = P["lns"].next()
        S.op("act", lambda e, st=st, pm=pm, tt=tt: e.activation(out=st[:, 0, :tt], in_=pm[:, :tt], func=AF.Copy), reads=[pm.k], writes=[st.k])
        S.op("dve", lambda e, st=st, tt=tt: e.tensor_tensor(out=st[:, 1, :tt], in0=st[:, 0, :tt], in1=st[:, 0, :tt], op=ALU.mult), reads=[st.k], writes=[st.k])
        S.op("dve", lambda e, st=st, pq=pq, tt=tt: e.tensor_tensor(out=st[:, 1, :tt], in0=pq[:, :tt], in1=st[:, 1, :tt], op=ALU.subtract), reads=[st.k, pq.k], writes=[st.k])
        S.op("act", lambda e, st=st, tt=tt: e.activation(out=st[:, 1, :tt], in_=st[:, 1, :tt], func=AF.Sqrt, bias=C.eps5[:, 0:1]), reads=[st.k, C.eps5.k], writes=[st.k])
        S.op("dve", lambda e, st=st, tt=tt: e.reciprocal(out=st[:, 1, :tt], in_=st[:, 1, :tt]), reads=[st.k], writes=[st.k])
        hb = P["x"].next() if hT is not None else None
        for kc in range(KC):
            S.op("dve", lambda e, zb=zb, st=st, kc=kc, tt=tt: e.tensor_tensor(out=zb[:, kc, :tt], in0=zb[:, kc, :tt], in1=st[:, 0, :tt], op=ALU.subtract), reads=[zb.k, st.k], writes=[zb.k])
            S.op("dve", lambda e, zb=zb, st=st, kc=kc, tt=tt: e.tensor_tensor(out=zb[:, kc, :tt], in0=zb[:, kc, :tt], in1=st[:, 1, :tt], op=ALU.mult), reads=[zb.k, st.k], writes=[zb.k])
            S.op("dve", lambda e, zb=zb, kc=kc, tt=tt: e.tensor_scalar(out=zb[:, kc, :tt], in0=zb[:, kc, :tt], scalar1=gT[:, kc:kc + 1], scalar2=bT[:, kc:kc + 1], op0=ALU.mult, op1=ALU.add), reads=[zb.k, gT.k, bT.k], writes=[zb.k])
            if hT is not None:
                S.op("dve", lambda e, zb=zb, hb=hb, kc=kc, tt=tt, col=col: e.tensor_scalar(out=hb[:, kc, :tt], in0=zb[:, kc, :tt], scalar1=C.mod[:, i, vsc * 32 + kc, col:col + 1], scalar2=C.mod[:, i, vsh * 32 + kc, col:col + 1], op0=ALU.mult, op1=ALU.add),
                     reads=[zb.k, C.mod.k], writes=[hb.k])
        S.dma_op("sp", lambda e, zb=zb, t0=t0, tt=tt: e.dma_start(out=xov[:, :, t0:t0 + tt], in_=zb[:, :, :tt]), reads=[zb.k], writes=[C.dk[xoname]])
        if hT is not None:
            S.dma_op("act", lambda e, hb=hb, t0=t0, tt=tt: e.dma_start(out=hv[:, :, t0:t0 + tt], in_=hb[:, :, :tt]), reads=[hb.k], writes=[C.dk[hname]])


def natten_tables(T):
    rows = (T - NCTX) // 64
    nb = rows // 2
    info = []
    for b in range(nb):
        sa = min(max(2 * b - 4, 0), rows - 8)
        R0 = min(sa, rows - 9)
        if b == 0: ty = 0
        elif b == 1: ty = 1
        elif b == nb - 2: ty = 3
        elif b == nb - 1: ty = 4
        else: ty = 2
        info.append((ty, R0))
    return info


def natten_bias_host(rpb, T):
    rows = (T - NCTX) // 64
    nb = rows // 2
    info = natten_tables(T)
    out = np.full((32, 5, 640, 128), -30000.0, np.float32)
    done = set()
    for b, (ty, R0) in enumerate(info):
        if ty in done:
            continue
        done.add(ty)
        qr = np.repeat(np.array([2 * b, 2 * b + 1]), 64)
        qc = np.tile(np.arange(64), 2)
        sr = np.clip(qr - 4, 0, rows - 8)
        sc = np.clip(qc - 8, 0, 64 - 16)
        kr = np.repeat(R0 + np.arange(9), 64)
        kc = np.tile(np.arange(64), 9)
        inwin = ((kr[:, None] >= sr[None]) & (kr[:, None] < sr[None] + 8) & (kc[:, None] >= sc[None]) & (kc[:, None] < sc[None] + 16))
        dr = np.clip(kr[:, None] - qr[None] + 7, 0, 14)
        dc = np.clip(kc[:, None] - qc[None] + 15, 0, 30)
        vals = rpb[:, dr, dc]
        out[:, ty, :576, :] = np.where(inwin[None], vals, np.float32(-30000.0))
    return out


def natten_attn(C, qkT, qkname, v, vname, biasT, oT, oname, need_ctx=True):
    S, P, nc, T = C.S, C.P, C.nc, C.T
    scale = 128 ** -0.5
    info = natten_tables(T)
    src = Tk("in", True)
    QB, KB = P["x"].bufs[0], P["x"].bufs[1]
    OB, BB = P["w"].bufs[0], P["w"].bufs[1]
    QT = QB.t[:].rearrange("p a b -> p (a b)")
    KT = KB.t[:].rearrange("p a b -> p (a b)")
    OH = OB.t[:].rearrange("p a b -> p (a b)")
    BT = BB.t[:].rearrange("p a b -> p (a b)").bitcast(F32)
    btv = BT[:, 0:3200].rearrange("p (y c q) -> p y c q", y=5, c=5)
    for h in range(32):
        S.dma_op("sp", lambda e, h=h: e.dma_start(out=QT[:, :T], in_=qkT[h * 128:(h + 1) * 128, :]), reads=[C.dk[qkname]], writes=[QB.k])
        S.dma_op("sp", lambda e, h=h: e.dma_start(out=KT[:, :T], in_=qkT[D + h * 128:D + (h + 1) * 128, :]), reads=[C.dk[qkname]], writes=[KB.k])
        S.dma_op("act", lambda e, h=h: e.dma_start(out=btv, in_=biasT[h].rearrange("y (c p) q -> p y c q", p=128)), reads=[src], writes=[BB.k])
        vc = P["vc"].next()
        S.dma_op("act", lambda e, h=h, vc=vc: e.dma_start(out=vc[:, :, :], in_=v[0:NCTX, h * 128:(h + 1) * 128].rearrange("(c p) d -> p c d", p=128)), reads=[C.dk[vname]], writes=[vc.k])
        blocks = [(-1, None)] if need_ctx else []
        blocks += list(enumerate(info))
        for b, inf in blocks:
            if b < 0:
                qoff, nq, chunks = 0, NCTX, [(0, 128, None, 0), (128, 128, None, 1)]
            else:
                ty, R0 = inf
                koff = NCTX + 64 * R0
                qoff, nq = NCTX + 128 * b, 128
                vb = P["vb"].next()
                S.dma_op("sp", lambda e, vb=vb, koff=koff, h=h: e.dma_start(out=vb[:, 0:4, :], in_=v[koff:koff + 512, h * 128:(h + 1) * 128].rearrange("(c p) d -> p c d", p=128)), reads=[C.dk[vname]], writes=[vb.k])
                S.dma_op("sp", lambda e, vb=vb, koff=koff, h=h: e.dma_start(out=vb[0:64, 4, :], in_=v[koff + 512:koff + 576, h * 128:(h + 1) * 128]), reads=[C.dk[vname]], writes=[vb.k])
                chunks = [(koff + 128 * c, 128 if c < 4 else 64, (ty, c), c) for c in range(5)] + [(0, 128, None, 5), (128, 128, None, 6)]
            po = P["pacc"].next()
            pd = P["pacc"].next()
            nch = len(chunks)
            for ci, (k0, kc, bias, vi) in enumerate(chunks):
                ps = P["ps"].next()
                S.op("pe", lambda e, ps=ps, k0=k0, kc=kc, qoff=qoff, nq=nq: e.matmul(ps[:kc, :nq], lhsT=KT[:, k0:k0 + kc], rhs=QT[:, qoff:qoff + nq], start=True, stop=True), reads=[QB.k, KB.k], writes=[ps.k])
                pT = P["pT"].next()
                if bias is not None:
                    sb = P["sb"].next()
                    S.op("dve", lambda e, sb=sb, ps=ps, kc=kc, bias=bias: e.scalar_tensor_tensor(out=sb[:kc, :128], in0=ps[:kc, :128], scalar=float(scale), in1=btv[:kc, bias[0], bias[1], :], op0=ALU.mult, op1=ALU.add), reads=[ps.k, BB.k], writes=[sb.k])
                    S.op("act", lambda e, sb=sb, pT=pT, kc=kc: e.activation(out=pT[:kc, :128], in_=sb[:kc, :128], func=AF.Exp), reads=[sb.k], writes=[pT.k])
                else:
                    S.op("act", lambda e, ps=ps, pT=pT, kc=kc, nq=nq: e.activation(out=pT[:kc, :nq], in_=ps[:kc, :nq], func=AF.Exp, scale=float(scale)), reads=[ps.k], writes=[pT.k])
                if b < 0 or vi >= 5:
                    vt, vk = vc[:kc, vi if b < 0 else vi - 5, :], vc.k
                else:
                    vt, vk = vb[:kc, vi, :], vb.k
                S.op("pe", lambda e, po=po, vt=vt, pT=pT, kc=kc, nq=nq, ci=ci: e.matmul(po[:, :nq], lhsT=vt, rhs=pT[:kc, :nq], start=(ci == 0), stop=(ci == nch - 1)), reads=[vk, pT.k], writes=[po.k])
                S.op("pe", lambda e, pd=pd, pT=pT, kc=kc, nq=nq, ci=ci: e.matmul(pd[:, :nq], lhsT=C.onesB[:kc, :], rhs=pT[:kc, :nq], start=(ci == 0), stop=(ci == nch - 1)), reads=[C.onesB.k, pT.k], writes=[pd.k])
            rc = P["rc"].next()
            S.op("dve", lambda e, rc=rc, pd=pd, nq=nq: e.reciprocal(out=rc[:, :nq], in_=pd[:, :nq]), reads=[pd.k], writes=[rc.k])
            if b < 0:
                oc = P["oc"].next()
                S.op("dve", lambda e, oc=oc, po=po, rc=rc, nq=nq: e.tensor_tensor(out=oc[:, :nq], in0=po[:, :nq], in1=rc[:, :nq], op=ALU.mult), reads=[po.k, rc.k], writes=[oc.k])
                S.dma_op("act", lambda e, oc=oc, h=h: e.dma_start(out=oT[h * 128:(h + 1) * 128, 0:NCTX], in_=oc[:, :NCTX]), reads=[oc.k], writes=[C.dk[oname]])
            else:
                S.op("dve", lambda e, po=po, rc=rc, b=b: e.tensor_tensor(out=OH[:, b * 128:(b + 1) * 128], in0=po[:, :128], in1=rc[:, :128], op=ALU.mult), reads=[po.k, rc.k], writes=[OB.k])
        S.dma_op("act", lambda e, h=h: e.dma_start(out=oT[h * 128:(h + 1) * 128, NCTX:T], in_=OH[:, 0:T - NCTX]), reads=[OB.k], writes=[C.dk[oname]])


NEG = -1.0e30


def bview(buf, dt=BF16):
    v = buf.t[:].rearrange("p a b -> p (a b)")
    return v if dt == BF16 else v.bitcast(dt)


def peer_topk(C, qT, qname, skT, tabT, tabname):
    S, P, nc, T = C.S, C.P, C.nc, C.T
    src = Tk("in", True)
    skb = C.skb
    S.dma_op("sp", lambda e: e.dma_start(out=skb[:], in_=skT), reads=[src], writes=[skb.k])
    qv = qT.rearrange("(c p) t -> p c t", p=128)
    for t0 in range(0, T, 128):
        qb = P["pq"].next()
        S.dma_op("sp", lambda e, qb=qb, t0=t0: e.dma_start(out=qb[:, :, :], in_=qv[:, :, t0:t0 + 128]), reads=[C.dk[qname]], writes=[qb.k])
        ssb = P["pss"].next()
        for ph in range(16):
            ps = P["ps"].next()
            S.op("pe", lambda e, ps=ps, qb=qb, ph=ph: e.matmul(ps[:, 0:96], lhsT=qb[:, ph, :], rhs=skb[:, ph, :], start=True, stop=True), reads=[qb.k, skb.k], writes=[ps.k])
            S.op("act", lambda e, ps=ps, ssb=ssb, ph=ph: e.activation(out=ssb[:, ph, :], in_=ps[:, 0:96], func=AF.Copy), reads=[ps.k], writes=[ssb.k])
        top = P["ptop"].next()
        tmp = P["ptmp"].next()
        for ph in range(16):
            S.op("dve", lambda e, top=top, ssb=ssb, ph=ph: e.max(out=top[:, ph, 0:8], in_=ssb[:, ph, :]), reads=[ssb.k], writes=[top.k])
            S.op("dve", lambda e, top=top, ssb=ssb, tmp=tmp, ph=ph: e.match_replace(out=tmp[:, 0:96], in_to_replace=top[:, ph, 0:8], in_values=ssb[:, ph, :], imm_value=NEG), reads=[ssb.k, top.k], writes=[tmp.k])
            S.op("dve", lambda e, top=top, tmp=tmp, ph=ph: e.max(out=top[:, ph, 8:16], in_=tmp[:, 0:96]), reads=[tmp.k], writes=[top.k])
        cand = P["pcand"].next()
        cv = cand.t[:].rearrange("p h (a b) -> p h a b", a=16)
        S.op("dve", lambda e, cv=cv, top=top: e.tensor_tensor(out=cv, in0=top[:, 0::2, :].unsqueeze(3).broadcast_to([128, 8, 16, 16]), in1=top[:, 1::2, :].unsqueeze(2).broadcast_to([128, 8, 16, 16]), op=ALU.add), reads=[top.k], writes=[cand.k])
        c16 = P["pc16"].next()
        for p in range(8):
            S.op("dve", lambda e, c16=c16, cand=cand, p=p: e.max(out=c16[:, p, 0:8], in_=cand[:, p, :]), reads=[cand.k], writes=[c16.k])
            S.op("dve", lambda e, c16=c16, cand=cand, tmp=tmp, p=p: e.match_replace(out=tmp[:, 0:256], in_to_replace=c16[:, p, 0:8], in_values=cand[:, p, :], imm_value=NEG), reads=[cand.k, c16.k], writes=[tmp.k])
            S.op("dve", lambda e, c16=c16, tmp=tmp, p=p: e.max(out=c16[:, p, 8:16], in_=tmp[:, 0:256]), reads=[tmp.k], writes=[c16.k])
        msk = P["pmsk"].next()
        for p in range(8):
            S.op("dve", lambda e, msk=msk, cand=cand, c16=c16, p=p: e.tensor_scalar(out=msk[:, p, :], in0=cand[:, p, :], scalar1=c16[:, p, 15:16], scalar2=None, op0=ALU.is_ge), reads=[cand.k, c16.k], writes=[msk.k])
        sm = P["psm"].next()
        Lb = P["pL"].next()
        S.op("dve", lambda e, Lb=Lb, msk=msk: e.tensor_reduce(out=Lb.t[:].rearrange("p h a -> p (h a)"), in_=msk.t[:].rearrange("p h (a b) -> p (h a) b", b=16), axis=AX.X, op=ALU.add), reads=[msk.k], writes=[Lb.k])
        S.op("dve", lambda e, sm=sm, c16=c16: e.tensor_scalar(out=sm[:, 0, :], in0=c16[:, :, 0], scalar1=-1.0, scalar2=None, op0=ALU.mult), reads=[c16.k], writes=[sm.k])
        ex = P["pex"].next()
        for p in range(8):
            S.op("act", lambda e, ex=ex, cand=cand, sm=sm, p=p: e.activation(out=ex[:, p, :], in_=cand[:, p, :], func=AF.Exp, bias=sm[:, 0, p:p + 1], scale=1.0), reads=[cand.k, sm.k], writes=[ex.k])
        S.op("dve", lambda e, ex=ex, msk=msk: e.tensor_tensor(out=ex[:, :, :], in0=ex[:, :, :], in1=msk[:, :, :], op=ALU.mult), reads=[ex.k, msk.k], writes=[ex.k])
        S.op("dve", lambda e, ex=ex, sm=sm: e.tensor_reduce(out=sm[:, 1, :], in_=ex[:, :, :], axis=AX.X, op=ALU.add), reads=[ex.k], writes=[sm.k])
        S.op("act", lambda e, sm=sm: e.activation(out=sm[:, 2, :], in_=sm[:, 1, :], func=AF.Ln), reads=[sm.k], writes=[sm.k])
        S.op("dve", lambda e, sm=sm, top=top: e.tensor_tensor(out=sm[:, 3, :], in0=top[:, 0::2, 0], in1=sm[:, 2, :], op=ALU.add), reads=[sm.k, top.k], writes=[sm.k])
        tf = P["ptf"].next()
        eq = P["peq"].next()
        for p in range(8):
            S.op("dve", lambda e, eq=eq, ssb=ssb, top=top, p=p: e.tensor_tensor(out=eq[:, :, :], in0=ssb[:, 2 * p, :].unsqueeze(2).broadcast_to([128, 96, 16]), in1=top[:, 2 * p, :].unsqueeze(1).broadcast_to([128, 96, 16]), op=ALU.is_equal), reads=[ssb.k, top.k], writes=[eq.k])
            S.op("dve", lambda e, eq=eq, Lb=Lb, p=p: e.tensor_tensor(out=eq[:, :, :], in0=eq[:, :, :], in1=Lb[:, p, :].unsqueeze(1).broadcast_to([128, 96, 16]), op=ALU.mult), reads=[eq.k, Lb.k], writes=[eq.k])
            S.op("dve", lambda e, eq=eq, tf=tf, p=p: e.tensor_reduce(out=tf[:, 0, p, 0, :], in_=eq[:, :, :], axis=AX.X, op=ALU.add), reads=[eq.k], writes=[tf.k])
            S.op("dve", lambda e, eq=eq, ssb=ssb, top=top, p=p: e.tensor_tensor(out=eq[:, :, :], in0=top[:, 2 * p + 1, :].unsqueeze(1).broadcast_to([128, 96, 16]), in1=ssb[:, 2 * p + 1, :].unsqueeze(2).broadcast_to([128, 96, 16]), op=ALU.is_gt), reads=[ssb.k, top.k], writes=[eq.k])
            S.op("dve", lambda e, eq=eq, tf=tf, p=p: e.tensor_reduce(out=tf[:, 1, p, 0, :], in_=eq[:, :, :], axis=AX.X, op=ALU.add, negate=True), reads=[eq.k], writes=[tf.k])
        S.op("dve", lambda e, tf=tf, ssb=ssb, sm=sm: e.tensor_tensor(out=tf[:, 0, :, 1, :], in0=ssb[:, 0::2, :], in1=sm[:, 3, :].unsqueeze(2).broadcast_to([128, 8, 96]), op=ALU.subtract), reads=[ssb.k, sm.k], writes=[tf.k])
        S.op("dve", lambda e, tf=tf, ssb=ssb, top=top: e.tensor_tensor(out=tf[:, 1, :, 1, :], in0=ssb[:, 1::2, :], in1=top[:, 1::2, 0:1].broadcast_to([128, 8, 96]), op=ALU.subtract), reads=[ssb.k, top.k], writes=[tf.k])
        tb = P["ptb"].next()
        S.op("dve", lambda e, tb=tb, tf=tf: e.tensor_copy(out=tb.t[:].rearrange("p w h k i -> p (w h k i)"), in_=tf.t[:].rearrange("p w h k i -> p (w h k i)")), reads=[tf.k], writes=[tb.k])
        tT = P["ptT"].next()
        for w in range(2):
            for p in range(8):
                ps = P["ps"].next()
                pv = ps.t[:, 0:128].bitcast(BF16)
                for k in range(2):
                    S.op("pe", lambda e, pv=pv, tb=tb, w=w, p=p, k=k: e.transpose(out=pv[0:96, k * 128:(k + 1) * 128], in_=tb[:, w, p, k, :], identity=C.identB[:, :]), reads=[tb.k, C.identB.k], writes=[ps.k])
                S.op("act", lambda e, pv=pv, tT=tT, w=w, p=p: e.activation(out=tT[0:96, w, p, :, :], in_=pv[0:96, 0:256].rearrange("i (k t) -> i k t", k=2), func=AF.Copy), reads=[ps.k], writes=[tT.k])
        S.dma_op("act", lambda e, tT=tT, t0=t0: e.dma_start(out=tabT[:, :, :, :, t0:t0 + 128], in_=tT[0:96, :, :, :, :]), reads=[tT.k], writes=[C.dk[tabname]])


def peer_g(C, tabT, tabname, E1d, E2d, GT, gname):
    S, P, T = C.S, C.P, C.T
    src = Tk("in", True)
    E1B, E2B = P["w"].bufs[0], P["w"].bufs[1]
    E1, E2 = bview(E1B), bview(E2B)
    S.dma_op("sp", lambda e: e.dma_start(out=E1[0:96, 0:9216], in_=E1d), reads=[src], writes=[E1B.k])
    S.dma_op("sp", lambda e: e.dma_start(out=E2[0:96, 0:9216], in_=E2d), reads=[src], writes=[E2B.k])
    groups = [(C.psA, [v.k for v in P["ps"].bufs]), (C.psB, [v.k for v in P["pacc"].bufs])]
    for t0 in range(0, T, 256):
        tb = P["gtab"].next()
        S.dma_op("sp", lambda e, tb=tb, t0=t0: e.dma_start(out=tb[0:96, :, :, :, :], in_=tabT[:, :, :, :, t0:t0 + 256]), reads=[C.dk[tabname]], writes=[tb.k])
        for ec in range(72):
            f = P["gf"].next()
            for half in range(2):
                pst, pks = groups[half]
                for j in range(4):
                    p = half * 4 + j
                    S.op("pe", lambda e, pst=pst, tb=tb, ec=ec, p=p, j=j: e.matmul(pst[:, j, :], lhsT=E1[0:96, ec * 128:(ec + 1) * 128], rhs=tb[0:96, 0, p, :, :], start=True, stop=False), reads=[tb.k, E1B.k], writes=[pks[j]])
                    S.op("pe", lambda e, pst=pst, tb=tb, ec=ec, p=p, j=j: e.matmul(pst[:, j, :], lhsT=E2[0:96, ec * 128:(ec + 1) * 128], rhs=tb[0:96, 1, p, :, :], start=False, stop=True), reads=[tb.k, E2B.k], writes=[pks[j]])
                eb = P["geb"].next()
                S.op("act", lambda e, pst=pst, eb=eb: e.activation(out=eb[:, :, :], in_=pst[:, :, 256:512], func=AF.Exp), reads=pks, writes=[eb.k])
                S.op("dve", lambda e, pst=pst, eb=eb, f=f, half=half: e.scalar_tensor_tensor(out=f[:, half * 4:half * 4 + 4, :], in0=pst[:, :, 0:256], scalar=0.5, in1=eb[:, :, :], op0=ALU.is_ge, op1=ALU.mult), reads=pks + [eb.k], writes=[f.k])
            gb = P["ob"].next()
            def red(e, f=f, gb=gb):
                with C.nc.allow_low_precision("sum of <=8 bf16 gates, fp32 internal"):
                    return e.tensor_reduce(out=gb[:, 0:256], in_=f.t[:].rearrange("p h t -> p t h"), axis=AX.X, op=ALU.add)
            S.op("dve", red, reads=[f.k], writes=[gb.k])
            S.dma_op("act", lambda e, gb=gb, ec=ec, t0=t0: e.dma_start(out=GT[ec * 128:(ec + 1) * 128, t0:t0 + 256], in_=gb[:, 0:256]), reads=[gb.k], writes=[C.dk[gname]])


def gelu_gate_epi(C, GT, gname, wT, wname):
    S, P = C.S, C.P
    def epi(ps, r0, t0, tt, col):
        gt = P["ob"].next()
        S.dma_op("sp", lambda e: e.dma_start(out=gt[:, :tt], in_=GT[r0:r0 + 128, t0:t0 + tt]), reads=[C.dk[gname]], writes=[gt.k])
        ga = P["o"].next()
        S.op("act", lambda e: e.activation(out=ga[:, :tt], in_=ps[:, :tt], func=AF.Gelu), reads=[ps.k], writes=[ga.k])
        wb_ = P["ob"].next()
        S.op("dve", lambda e: e.tensor_tensor(out=wb_[:, :tt], in0=ga[:, :tt], in1=gt[:, :tt], op=ALU.mult), reads=[ga.k, gt.k], writes=[wb_.k])
        S.dma_op("act", lambda e: e.dma_start(out=wT[r0:r0 + 128, t0:t0 + tt], in_=wb_[:, :tt]), reads=[wb_.k], writes=[C.dk[wname]])
    return epi


def linear_fm_bigk(C, xT, xname, W, K, N, epi, kg=24):
    S, P = C.S, C.P
    kcn = K // 128
    ngrp = kcn // kg
    xv = xT.rearrange("(kc p) t -> p kc t", p=128)
    wv = W.rearrange("(kc p) n -> p kc n", p=128)
    src = Tk("in", True)
    for n0 in range(0, N, 512):
        for (t0, tt, col) in C.tiles(512):
            pss = [P["pacc"].next() for _ in range(4)]
            for g in range(ngrp):
                wb = P["w"].next()
                xb = P["x"].next()
                S.dma_op("pool", lambda e, wb=wb, n0=n0, g=g: e.dma_start(out=wb[:, :kg, :], in_=wv[:, g * kg:(g + 1) * kg, n0:n0 + 512]), reads=[src], writes=[wb.k])
                S.dma_op("sp", lambda e, xb=xb, t0=t0, tt=tt, g=g: e.dma_start(out=xb[:, :kg, :tt], in_=xv[:, g * kg:(g + 1) * kg, t0:t0 + tt]), reads=[C.dk[xname]], writes=[xb.k])
                for j in range(4):
                    for kc in range(kg):
                        S.op("pe", lambda e, ps=pss[j], wb=wb, xb=xb, kc=kc, j=j, tt=tt, g=g: e.matmul(ps[:, :tt], lhsT=wb[:, kc, j * 128:(j + 1) * 128], rhs=xb[:, kc, :tt], start=(g == 0 and kc == 0), stop=(g == ngrp - 1 and kc == kg - 1)),
                             reads=[wb.k, xb.k], writes=[pss[j].k])
            for j in range(4):
                epi(pss[j], n0 + j * 128, t0, tt, col)


def setup_pools(C):
    nc, P = C.nc, C.P
    X0, X1, W0, W1 = P["x"].bufs[0], P["x"].bufs[1], P["w"].bufs[0], P["w"].bufs[1]
    C.skb = Buf(nc, "skb", [128, 16, 96], F32)
    C.identB = Buf(nc, "identB", [128, 128], BF16)
    C.onesB = Buf(nc, "onesB", [128, 128], BF16)
    C.onesD = Buf(nc, "onesD", [128, 128], F32)
    C.eps5 = Buf(nc, "eps5", [128, 1], F32)
    C.S.op("dve", lambda e: e.memset(C.onesB[:], 1.0), writes=[C.onesB.k])
    C.S.op("dve", lambda e: e.memset(C.onesD[:], 1.0 / D), writes=[C.onesD.k])
    C.S.op("dve", lambda e: e.memset(C.eps5[:], 1e-5), writes=[C.eps5.k])
    ptf, pmsk, pex = carve(X0, F32, [(2, 8, 2, 96), (8, 256), (8, 256)])
    pcand, peq, pq, pss = carve(X1, F32, [(8, 256), (96, 16), (16, 128), (16, 96)])
    P["ptf"], P["pmsk"], P["pex"] = VRing([ptf]), VRing([pmsk]), VRing([pex])
    P["pcand"], P["peq"], P["pq"], P["pss"] = VRing([pcand]), VRing([peq]), VRing([pq]), VRing([pss])
    P["ptop"] = Ring(nc, "ptop", [128, 16, 16], F32, 2)
    P["ptmp"] = Ring(nc, "ptmp", [128, 256], F32, 2)
    P["pc16"] = Ring(nc, "pc16", [128, 8, 16], F32, 2)
    P["psm"] = Ring(nc, "psm", [128, 4, 8], F32, 2)
    P["pL"] = Ring(nc, "pL", [128, 8, 16], F32, 2)
    ptb, ptT = carve(W0, BF16, [(2, 8, 2, 96), (2, 8, 2, 128)])
    P["ptb"], P["ptT"] = VRing([ptb]), VRing([ptT])
    g0, = carve(X0, BF16, [(2, 8, 2, 256)])
    g1, = carve(X1, BF16, [(2, 8, 2, 256)])
    P["gtab"] = VRing([g0, g1])
    P["geb"] = Ring(nc, "geb", [128, 4, 256], F32, 1)
    P["gf"] = Ring(nc, "gf", [128, 8, 256], BF16, 1)
    z0, = carve(W0, F32, [(KC, 256)])
    zq, = carve(W1, F32, [(KC, 256)])
    P["lnz"], P["lnq"] = VRing([z0]), VRing([zq])
    P["lns"] = Ring(nc, "lns", [128, 2, 256], F32, 2)
    f0, f1 = carve(W1, F32, [(16, 128), (16, 128)])
    P["xf"] = VRing([f0, f1])
    C.ones1 = Buf(nc, "ones1", [128, 128], F32)
    C.epsn = Buf(nc, "epsn", [128, 1], F32)
    C.S.op("dve", lambda e: e.memset(C.ones1[:], 1.0), writes=[C.ones1.k])
    C.S.op("dve", lambda e: e.memset(C.epsn[:], 1e-6), writes=[C.epsn.k])
    C.gk = Tk("gcols")
    n0, n1, n2 = carve(W0, F32, [(8, 256), (8, 256), (8, 256)])
    P["nx"], P["nq"] = VRing([n0, n1]), VRing([n2])
    P["nst"] = Ring(nc, "nst", [128, 256], F32, 2)
    b0, b1 = carve(W1, BF16, [(8, 256), (8, 256)])
    P["nob"] = VRing([b0, b1])
    P["nct"] = Ring(nc, "nct", [128, 2, 256], F32, 2)
    P["vc"] = Ring(nc, "vc", [128, 2, 128], BF16, 2)
    P["vb"] = Ring(nc, "vb", [128, 5, 128], BF16, 3)
    P["pT"] = Ring(nc, "pT", [128, 512], BF16, 4)
    P["sb"] = Ring(nc, "sb", [128, 128], F32, 3)
    P["rc"] = Ring(nc, "rc", [128, 512], F32, 2)
    P["oc"] = Ring(nc, "oc", [128, 512], BF16, 2)


def norm_stage(C, srcT, sname, F, dstT, dname, gcol=None, eps=1e-6, do_norm=True, rope=None, src_row0=0, dst_row0=0):
    S, P, T = C.S, C.P, C.T
    nch = (F + 127) // 128
    pr = min(F, 128)
    src = Tk("in", True)
    for t0 in range(0, T, 256):
        tt = min(256, T - t0)
        xb = P["nx"].next()
        for c in range(nch):
            S.dma_op("sp", lambda e, xb=xb, c=c, t0=t0, tt=tt: e.dma_start(out=xb[:pr, c, :tt], in_=srcT[src_row0 + c * 128:src_row0 + c * 128 + pr, t0:t0 + tt]), reads=[C.dk[sname]], writes=[xb.k])
        if do_norm:
            sq = P["nq"].next()
            S.op("act", lambda e, xb=xb, sq=sq, tt=tt: e.activation(out=sq[:pr, :nch, :tt], in_=xb[:pr, :nch, :tt], func=AF.Square), reads=[xb.k], writes=[sq.k])
            pm = P["ps"].next()
            for c in range(nch):
                S.op("pe", lambda e, pm=pm, sq=sq, c=c, tt=tt: e.matmul(pm[:pr, :tt], lhsT=C.ones1[:pr, :pr], rhs=sq[:pr, c, :tt], start=(c == 0), stop=(c == nch - 1)), reads=[sq.k, C.ones1.k], writes=[pm.k])
            st = P["nst"].next()
            S.op("act", lambda e, st=st, pm=pm, tt=tt: e.activation(out=st[:pr, :tt], in_=pm[:pr, :tt], func=AF.Sqrt, bias=C.epsn[:pr, 0:1], scale=1.0 / F), reads=[pm.k, C.epsn.k], writes=[st.k])
            S.op("dve", lambda e, st=st, tt=tt: e.reciprocal(out=st[:pr, :tt], in_=st[:pr, :tt]), reads=[st.k], writes=[st.k])
            for c in range(nch):
                S.op("dve", lambda e, xb=xb, st=st, c=c, tt=tt: e.tensor_tensor(out=xb[:pr, c, :tt], in0=xb[:pr, c, :tt], in1=st[:pr, :tt], op=ALU.mult), reads=[xb.k, st.k], writes=[xb.k])
                S.op("dve", lambda e, xb=xb, c=c, tt=tt: e.tensor_scalar(out=xb[:pr, c, :tt], in0=xb[:pr, c, :tt], scalar1=gcol[:pr, c:c + 1], scalar2=None, op0=ALU.mult), reads=[xb.k, C.gk], writes=[xb.k])
        ob = P["nob"].next()
        if rope is not None:
            CtT, StT, Rm = rope
            ct = P["nct"].next()
            S.dma_op("act", lambda e, ct=ct, t0=t0, tt=tt: e.dma_start(out=ct[:pr, 0, :tt], in_=CtT[:, t0:t0 + tt]), reads=[src], writes=[ct.k])
            S.dma_op("act", lambda e, ct=ct, t0=t0, tt=tt: e.dma_start(out=ct[:pr, 1, :tt], in_=StT[:, t0:t0 + tt]), reads=[src], writes=[ct.k])
            pr_ = P["ps"].next()
            S.op("pe", lambda e, pr_=pr_, xb=xb, tt=tt: e.matmul(pr_[:pr, :tt], lhsT=Rm[:pr, :pr], rhs=xb[:pr, 0, :tt], start=True, stop=True), reads=[xb.k, Rm.k], writes=[pr_.k])
            S.op("dve", lambda e, ct=ct, pr_=pr_, tt=tt: e.tensor_tensor(out=ct[:pr, 1, :tt], in0=pr_[:pr, :tt], in1=ct[:pr, 1, :tt], op=ALU.mult), reads=[pr_.k, ct.k], writes=[ct.k])
            S.op("dve", lambda e, ct=ct, xb=xb, tt=tt: e.tensor_tensor(out=ct[:pr, 0, :tt], in0=xb[:pr, 0, :tt], in1=ct[:pr, 0, :tt], op=ALU.mult), reads=[xb.k, ct.k], writes=[ct.k])
            S.op("dve", lambda e, ct=ct, ob=ob, tt=tt: e.tensor_tensor(out=ob[:pr, 0, :tt], in0=ct[:pr, 0, :tt], in1=ct[:pr, 1, :tt], op=ALU.add), reads=[ct.k], writes=[ob.k])
        else:
            S.op("act", lambda e, xb=xb, ob=ob, tt=tt: e.activation(out=ob[:pr, :nch, :tt], in_=xb[:pr, :nch, :tt], func=AF.Copy), reads=[xb.k], writes=[ob.k])
        for c in range(nch):
            S.dma_op("sp", lambda e, ob=ob, c=c, t0=t0, tt=tt: e.dma_start(out=dstT[dst_row0 + c * 128:dst_row0 + c * 128 + pr, t0:t0 + tt], in_=ob[:pr, c, :tt]), reads=[ob.k], writes=[C.dk[dname]])


def dense_attn(C, nheads, qparts, kparts, vsrc, scale, oT, oname, need_ctx, names):
    S, P, T = C.S, C.P, C.T
    X0, X1, W0, W1 = P["x"].bufs[0], P["x"].bufs[1], P["w"].bufs[0], P["w"].bufs[1]
    kvs = [(bview(X0), X0.k), (bview(X1), X1.k)]
    qv = bview(W1)
    vv = bview(W0)[:, 0:66 * 128].rearrange("p (c d) -> p c d", d=128)
    nkc_all = T // 128
    rd = [C.dk[n] for n in names]
    for h in range(nheads):
        qp, kp = qparts(h), kparts(h)
        dks = [qa.shape[0] for qa in qp]
        for i, ka in enumerate(kp):
            S.dma_op("sp", lambda e, ka=ka, i=i, dk=dks[i]: e.dma_start(out=kvs[i][0][:dk, 0:T], in_=ka), reads=rd, writes=[kvs[i][1]])
        va = vsrc(h)
        S.dma_op("act", lambda e, va=va: e.dma_start(out=vv[:, 0:nkc_all, :], in_=va.rearrange("(c p) d -> p c d", p=128)), reads=rd, writes=[W0.k])
        qtiles = ([(0, NCTX, 2)] if need_ctx else []) + [(t0, tt, nkc_all) for (t0, tt, col) in C.tiles(512) if col == 0]
        for (q0, nq, nkc) in qtiles:
            for i, qa in enumerate(qp):
                S.dma_op("sp", lambda e, qa=qa, i=i, dk=dks[i], q0=q0, nq=nq: e.dma_start(out=qv[:dk, i * 512:i * 512 + nq], in_=qa[:, q0:q0 + nq]), reads=rd, writes=[W1.k])
            po = P["pacc"].next()
            pd = P["pacc"].next()
            for kc in range(nkc):
                ps = P["ps"].next()
                for i, dk in enumerate(dks):
                    S.op("pe", lambda e, ps=ps, dk=dk, kc=kc, nq=nq, i=i: e.matmul(ps[:, :nq], lhsT=kvs[i][0][:dk, kc * 128:(kc + 1) * 128], rhs=qv[:dk, i * 512:i * 512 + nq], start=(i == 0), stop=(i == len(dks) - 1)),
                         reads=[kvs[i][1], W1.k], writes=[ps.k])
                pT = P["pT"].next()
                S.op("act", lambda e, ps=ps, pT=pT, nq=nq: e.activation(out=pT[:, :nq], in_=ps[:, :nq], func=AF.Exp, scale=float(scale)), reads=[ps.k], writes=[pT.k])
                S.op("pe", lambda e, po=po, pT=pT, kc=kc, nq=nq, nkc=nkc: e.matmul(po[:, :nq], lhsT=vv[:, kc, :], rhs=pT[:, :nq], start=(kc == 0), stop=(kc == nkc - 1)), reads=[W0.k, pT.k], writes=[po.k])
                S.op("pe", lambda e, pd=pd, pT=pT, kc=kc, nq=nq, nkc=nkc: e.matmul(pd[:, :nq], lhsT=C.onesB[:, :], rhs=pT[:, :nq], start=(kc == 0), stop=(kc == nkc - 1)), reads=[C.onesB.k, pT.k], writes=[pd.k])
            rc = P["rc"].next()
            S.op("dve", lambda e, rc=rc, pd=pd, nq=nq: e.reciprocal(out=rc[:, :nq], in_=pd[:, :nq]), reads=[pd.k], writes=[rc.k])
            oc = P["oc"].next()
            S.op("dve", lambda e, oc=oc, po=po, rc=rc, nq=nq: e.tensor_tensor(out=oc[:, :nq], in0=po[:, :nq], in1=rc[:, :nq], op=ALU.mult), reads=[po.k, rc.k], writes=[oc.k])
            S.dma_op("act", lambda e, oc=oc, h=h, q0=q0, nq=nq: e.dma_start(out=oT[h * 128:(h + 1) * 128, q0:q0 + nq], in_=oc[:, :nq]), reads=[oc.k], writes=[C.dk[oname]])


def rope_tables_host(T, F):
    s = T - NCTX
    t = np.arange(s)
    rows, cols = (t // 64).astype(np.float32), (t % 64).astype(np.float32)
    h = F // 2
    half = h // 2
    inv = (10000.0 ** (-np.arange(half, dtype=np.float32) / half)).astype(np.float32)
    Ct = np.ones((F, T), np.float32)
    St = np.zeros((F, T), np.float32)
    R = np.zeros((F, F), np.float32)
    for blk, pos in enumerate((rows, cols)):
        ang = pos[None, :] * inv[:, None]
        c, sn = np.cos(ang).astype(np.float32), np.sin(ang).astype(np.float32)
        b0 = blk * h
        Ct[b0:b0 + half, NCTX:] = c
        Ct[b0 + half:b0 + h, NCTX:] = c
        St[b0:b0 + half, NCTX:] = -sn
        St[b0 + half:b0 + h, NCTX:] = sn
        for j in range(half):
            R[b0 + half + j, b0 + j] = 1.0
            R[b0 + j, b0 + half + j] = 1.0
    return Ct, St, R


def build_program(T, layers):
    nc = bass.Bass("TRN2", target_bir_lowering=False)
    def din(name, shape, dt=F32):
        return nc.dram_tensor(name, list(shape), dt, kind="ExternalInput").ap()
    C = Ctx(nc, T)
    S, P = C.S, C.P
    setup_pools(C)
    src = Tk("in", True)
    L = len(layers)
    xT = din("xT", [D, T]); C.dk["xT"] = Tk("xT", True)
    ada_w = din("ada_w", [L, D, 6 * D])
    ada_bT = din("ada_bT", [128, 4, 192])
    ccT = din("ccT", [128, KC, 2])
    lnv_d = din("lnv", [128, L, 2, 2, KC])
    vecs_d = din("vecs", [128, 16])
    ident_d = din("identd", [128, 128], BF16)
    E1d = din("E1d", [96, 9216], BF16); E2d = din("E2d", [96, 9216], BF16)
    ct128 = din("ct128", [128, T]); st128 = din("st128", [128, T]); r128_d = din("r128", [128, 128])
    ct64 = din("ct64", [64, T]); st64 = din("st64", [64, T]); r64_d = din("r64", [64, 64])
    out = nc.dram_tensor("out", [D, T], F32, kind="ExternalOutput").ap(); C.dk["out"] = Tk("out", True)
    lnv = Buf(nc, "lnv_sb", [128, L, 2, 2, KC], F32)
    vecs = Buf(nc, "vecs_sb", [128, 16], F32); C.gk = vecs.k
    R128 = Buf(nc, "R128", [128, 128], F32); R64 = Buf(nc, "R64", [64, 64], F32)
    for b, d_ in ((lnv, lnv_d), (vecs, vecs_d), (C.identB, ident_d), (R128, r128_d), (R64, r64_d)):
        S.dma_op("sp", lambda e, b=b, d_=d_: e.dma_start(out=b[:], in_=d_), reads=[src], writes=[b.k])
    hT = C.dram("hT", [D, T], BF16); qkT = C.dram("qkT", [2 * D, T], BF16); vtm = C.dram("vtm", [T, 2 * D], BF16)
    oT = C.dram("oT", [D, T], BF16); zT = C.dram("zT", [D, T], F32); x1T = C.dram("x1T", [D, T], F32)
    h2T = C.dram("h2T", [D, T], BF16); pqT = C.dram("pqT", [2048, T], F32); tabT = C.dram("tabT", [96, 2, 8, 2, T], BF16)
    GT = C.dram("GT", [9216, T], BF16); wT = C.dram("wT", [9216, T], BF16); z2T = C.dram("z2T", [D, T], F32)
    xa = C.dram("xa", [D, T], F32); xb_ = C.dram("xb", [D, T], F32)
    rawT = C.dram("rawT", [2048, T], F32); rawQ = C.dram("rawQ", [6144, T], F32); nrmT = C.dram("nrmT", [1536, T], BF16); kpeT = C.dram("kpeT", [64, T], BF16)
    qpeT = C.dram("qpeT", [32 * 64, T], BF16)
    kvT = C.dram("kvT", [8192, T], BF16)
    ada_stage_layers(C, ada_w, ada_bT, ccT, L)
    cur, curname = xT, "xT"
    for li, i in enumerate(layers):
        kind = i % 3
        modulate(C, cur, curname, hT, "hT", li, 0, 1)
        if kind == 0:
            wqkv = din(f"wqkv{i}", [D, 3 * D]); biasT = din(f"biasT{i}", [32, 5, 640, 128]); wo = din(f"wo{i}", [D, D])
            linear_fm(C, hT, "hT", wqkv[:, 0:2 * D], qkT, "qkT", D, 2 * D)
            linear_tm(C, hT, "hT", wqkv[:, 2 * D:3 * D], vtm[:, 0:D], "vtm", D, D)
            natten_attn(C, qkT, "qkT", vtm[:, 0:D], "vtm", biasT, oT, "oT", need_ctx=True)
        elif kind == 1:
            wdq = din(f"wdq{i}", [D, 1024]); wuq = din(f"wuq{i}", [1024, 6144]); wdkv = din(f"wdkv{i}", [D, 576])
            wukv = din(f"wukv{i}", [512, 8192]); wo = din(f"wo{i}", [D, D])
            linear_fm(C, hT, "hT", wdq, rawT[0:1024], "rawT", D, 1024, ybf16=False)
            linear_fm(C, hT, "hT", wdkv, rawT[1024:1600], "rawT", D, 576, ybf16=False)
            norm_stage(C, rawT, "rawT", 1024, nrmT, "nrmT", gcol=vecs[:, 0:8])
            norm_stage(C, rawT, "rawT", 512, nrmT, "nrmT", gcol=vecs[:, 8:12], src_row0=1024, dst_row0=1024)
            norm_stage(C, rawT, "rawT", 64, kpeT, "kpeT", do_norm=False, rope=(ct64, st64, R64), src_row0=1536)
            linear_fm(C, nrmT[0:1024], "nrmT", wuq, rawQ, "rawQ", 1024, 6144, ybf16=False)
            for h in range(32):
                norm_stage(C, rawQ, "rawQ", 128, qkT, "qkT", do_norm=False, src_row0=192 * h, dst_row0=128 * h)
                norm_stage(C, rawQ, "rawQ", 64, qpeT, "qpeT", do_norm=False, rope=(ct64, st64, R64), src_row0=192 * h + 128, dst_row0=64 * h)
            linear_fm(C, nrmT[1024:1536], "nrmT", wukv, kvT, "kvT", 512, 8192)
            linear_tm(C, nrmT[1024:1536], "nrmT", wukv, vtm, "vtm", 512, 8192)
            kv_ = kvT
            dense_attn(C, 32, lambda h: [qkT[128 * h:128 * h + 128], qpeT[64 * h:64 * h + 64]],
                       lambda h: [kv_[256 * h:256 * h + 128], kpeT[0:64]],
                       lambda h: vtm[:, 256 * h + 128:256 * h + 256], 192 ** -0.5, oT, "oT", True, ["qkT", "qpeT", "kvT", "kpeT", "vtm"])
        else:
            wq = din(f"wq{i}", [D, D]); wk = din(f"wk{i}", [D, 1024]); wv = din(f"wv{i}", [D, 1024]); wo = din(f"wo{i}", [D, D])
            linear_fm(C, hT, "hT", wq, rawQ[0:D], "rawQ", D, D, ybf16=False)
            linear_fm(C, hT, "hT", wk, rawT[0:1024], "rawT", D, 1024, ybf16=False)
            linear_tm(C, hT, "hT", wv, vtm[:, 0:1024], "vtm", D, 1024)
            for h in range(32):
                norm_stage(C, rawQ, "rawQ", 128, qkT, "qkT", gcol=vecs[:, 12:13], rope=(ct128, st128, R128), src_row0=128 * h, dst_row0=128 * h)
            for g in range(8):
                norm_stage(C, rawT, "rawT", 128, qkT, "qkT", gcol=vecs[:, 13:14], rope=(ct128, st128, R128), src_row0=128 * g, dst_row0=D + 128 * g)
            dense_attn(C, 32, lambda h: [qkT[128 * h:128 * h + 128]], lambda h: [qkT[D + 128 * (h // 4):D + 128 * (h // 4) + 128]],
                       lambda h: vtm[:, 128 * (h // 4):128 * (h // 4) + 128], 128 ** -0.5, oT, "oT", True, ["qkT", "vtm"])
        linear_fm(C, oT, "oT", wo, None, None, D, D, epi=resid_epi(C, cur, curname, zT, "zT", li, 2))
        layernorm(C, zT, "zT", x1T, "x1T", View(lnv[:, li, 0, 0, :], lnv.k), View(lnv[:, li, 0, 1, :], lnv.k), hT=h2T, hname="h2T", i=li, vsh=3, vsc=4)
        wqr = din(f"wqr{i}", [D, 2048]); skT = din(f"skT{i}", [128, 16, 96]); uT = din(f"uT{i}", [D, 9216]); pv = din(f"pv{i}", [9216, D])
        linear_fm(C, h2T, "h2T", wqr, pqT, "pqT", D, 2048, ybf16=False)
        peer_topk(C, pqT, "pqT", skT, tabT, "tabT")
        peer_g(C, tabT, "tabT", E1d, E2d, GT, "GT")
        linear_fm(C, h2T, "h2T", uT, None, None, D, 9216, epi=gelu_gate_epi(C, GT, "GT", wT, "wT"))
        linear_fm_bigk(C, wT, "wT", pv, 9216, D, epi=resid_epi(C, x1T, "x1T", z2T, "z2T", li, 5))
        last = (li == L - 1)
        nxt, nname = (out, "out") if last else ((xa, "xa") if li % 2 == 0 else (xb_, "xb"))
        layernorm(C, z2T, "z2T", nxt, nname, View(lnv[:, li, 1, 0, :], lnv.k), View(lnv[:, li, 1, 1, :], lnv.k))
        cur, curname = nxt, nname
    S.final_wait([C.dk["out"]])
    S.emit()
    print("nops", S.nops, "nsem", S.nsem, flush=True)
    return nc


def ada_stage_layers(C, ada_w, ada_bT, ccT, L):
    ada_stage(C, ada_w, ada_bT, ccT, layers=L)


def host_inputs(inp, T, layers):
    f32 = lambda a: np.ascontiguousarray(np.asarray(a, np.float32))
    x = f32(inp["x"])[0]; ctx = f32(inp["ctx"])[0]
    L = len(layers)
    d = {}
    d["xT"] = f32(np.concatenate([ctx, x], 0).T)
    d["ada_w"] = f32(np.asarray(inp["ada_w"])[layers])
    ab = np.zeros((4, 6 * D), np.float32); ab[:L] = f32(inp["ada_b"])[layers]
    d["ada_bT"] = f32(ab.reshape(4, 192, 128).transpose(2, 0, 1))
    cc = np.stack([f32(inp["c"])[0], f32(inp["c_ctx"])], 0)
    d["ccT"] = f32(cc.reshape(2, KC, 128).transpose(2, 1, 0))
    g = f32(inp["ln_g"])[layers]; b = f32(inp["ln_b"])[layers]
    lnv = np.stack([g, b], 2).reshape(L, 2, 2, KC, 128)
    d["lnv"] = f32(lnv.transpose(4, 0, 1, 2, 3))
    vecs = np.zeros((128, 16), np.float32)
    vecs[:, 0:8] = f32(inp["mla_q_norm"])[0].reshape(8, 128).T
    vecs[:, 8:12] = f32(inp["mla_kv_norm"])[0].reshape(4, 128).T
    vecs[:, 12] = f32(inp["gqa_q_norm"])[0]; vecs[:, 13] = f32(inp["gqa_k_norm"])[0]
    d["vecs"] = vecs
    d["identd"] = np.eye(128).astype(ml_dtypes.bfloat16)
    e = np.arange(9216)
    d["E1d"] = (e[None] // 96 == np.arange(96)[:, None]).astype(ml_dtypes.bfloat16)
    d["E2d"] = (e[None] % 96 == np.arange(96)[:, None]).astype(ml_dtypes.bfloat16)
    d["ct128"], d["st128"], d["r128"] = rope_tables_host(T, 128)
    d["ct64"], d["st64"], d["r64"] = rope_tables_host(T, 64)
    for i in layers:
        kind, j = i % 3, i // 3
        if kind == 0:
            d[f"wqkv{i}"] = f32(inp["na_w_qkv"][j]); d[f"biasT{i}"] = natten_bias_host(f32(inp["na_rpb"][j]), T); d[f"wo{i}"] = f32(inp["na_w_o"][j])
        elif kind == 1:
            d[f"wdq{i}"] = f32(inp["mla_w_dq"][j]); d[f"wuq{i}"] = f32(inp["mla_w_uq"][j]); d[f"wdkv{i}"] = f32(inp["mla_w_dkv"][j])
            d[f"wukv{i}"] = f32(inp["mla_w_ukv"][j]); d[f"wo{i}"] = f32(inp["mla_w_o"][j])
        else:
            d[f"wq{i}"] = f32(inp["gqa_w_q"][j]); d[f"wk{i}"] = f32(inp["gqa_w_k"][j]); d[f"wv{i}"] = f32(inp["gqa_w_v"][j]); d[f"wo{i}"] = f32(inp["gqa_w_o"][j])
        d[f"wqr{i}"] = f32(inp["peer_w_query"][i])
        d[f"skT{i}"] = f32(f32(inp["peer_sub_keys"][i]).reshape(16, 96, 128).transpose(2, 0, 1))
        d[f"uT{i}"] = f32(f32(inp["peer_u"][i]).T)
        d[f"pv{i}"] = f32(inp["peer_v"][i])
    return d


T_ALL = NCTX + 8192
LAYERS = [0, 1, 2, 3]


def kernel(**inputs):
    import time
    t0 = time.time()
    xT = None
    for i in LAYERS:
        ins = host_inputs(inputs, T_ALL, [i])
        if xT is not None:
            ins["xT"] = xT
        nc = build_program(T_ALL, [i])
        print(f"[kernel] layer {i}: built at {time.time() - t0:.0f}s", flush=True)
        res = run_bass_kernel_spmd(nc, [ins], core_ids=[0])
        xT = np.ascontiguousarray(res.results[0]["out"])
        print(f"[kernel] layer {i}: done at {time.time() - t0:.0f}s", flush=True)
        del ins, nc, res
    return np.ascontiguousarray(xT[:, NCTX:].T)[None].astype(np.float32)
```
